# Optimizing a Trainium2 kernel written in Bass

```python
import jax
import jax.numpy as jnp
from jax import lax
import numpy as np

D_MODEL = 2048
BATCH = 1
SEQ = 8192
DEPTH = 4

A_HEADS = 16
A_KV_GROUPS = 4
A_HEADS_PER_GROUP = A_HEADS // A_KV_GROUPS
A_HEAD_DIM = D_MODEL // A_HEADS
A_WIDTH = A_HEADS * A_HEAD_DIM
A_KV_WIDTH = A_KV_GROUPS * A_HEAD_DIM
A_N_BRANCH = 3
CMP_BLOCK = 32
CMP_STRIDE = 16
CMP_HIDDEN = 256
SEL_BLOCK = 64
SEL_TOP_N = 16
SEL_Q_CHUNK = 64
WIN_SIZE = 512
WIN_Q_BLOCK = 128
B_HEADS = 4
B_KEY_WIDTH = D_MODEL // 2
B_VAL_WIDTH = D_MODEL
B_KEY_DIM = B_KEY_WIDTH // B_HEADS
B_VAL_DIM = B_VAL_WIDTH // B_HEADS
B_ALPHA_RANK = 16
B_GATE_TEMP = 16.0
B_CHUNK = 64
NORM_EPS = 1e-6

IN_SIZES = (A_WIDTH, A_KV_WIDTH, A_KV_WIDTH, A_KV_WIDTH, A_KV_WIDTH, A_KV_WIDTH, A_KV_WIDTH,
            A_HEADS * A_N_BRANCH, A_WIDTH,
            B_KEY_WIDTH, B_KEY_WIDTH, B_VAL_WIDTH, B_ALPHA_RANK, B_VAL_WIDTH,
            D_MODEL, D_MODEL)
IN_COLS = sum(IN_SIZES)

kernel_name = 'hybrid_nsa_gla_gated_block'


def rms_norm(x, w):
    xf = x.astype(jnp.float32)
    y = xf * lax.rsqrt(jnp.mean(xf * xf, axis=-1, keepdims=True) + NORM_EPS)
    return (y * w.astype(jnp.float32)).astype(x.dtype)


def masked_softmax(s, mask):
    s = jnp.where(mask, s.astype(jnp.float32), -jnp.inf)
    m = jnp.max(s, axis=-1, keepdims=True)
    m = jnp.where(jnp.isfinite(m), m, 0.0)
    p = jnp.exp(s - m)
    denom = jnp.sum(p, axis=-1, keepdims=True)
    return p / jnp.where(denom > 0, denom, 1.0)


def compress_tokens(kv, pe, w1, b1, w2, b2):
    B, T, G, DH = kv.shape
    ratio = CMP_BLOCK // CMP_STRIDE
    c = kv.reshape(B, T // CMP_STRIDE, CMP_STRIDE, G, DH)
    nc = T // CMP_STRIDE - ratio + 1
    blocks = jnp.concatenate([c[:, r:r + nc] for r in range(ratio)], axis=2)
    blocks = blocks + pe[None, None, :, None, :]
    flat = blocks.transpose(0, 1, 3, 2, 4).reshape(B, nc, G, CMP_BLOCK * DH)
    hid = jax.nn.gelu(flat @ w1 + b1)
    return hid @ w2 + b2


def selected_attention(q, k_sel, v_sel, idx):
    B, T, G, HG, DH = q.shape
    n = idx.shape[-1]
    nb = T // SEL_BLOCK
    kb = k_sel.reshape(B, nb, SEL_BLOCK, G, DH).transpose(0, 3, 1, 2, 4)
    vb = v_sel.reshape(B, nb, SEL_BLOCK, G, DH).transpose(0, 3, 1, 2, 4)
    nqc = T // SEL_Q_CHUNK
    q_c = jnp.moveaxis(q.reshape(B, nqc, SEL_Q_CHUNK, G, HG, DH), 1, 0)
    i_c = jnp.moveaxis(idx.reshape(B, nqc, SEL_Q_CHUNK, G, n), 1, 0)
    t_c = jnp.arange(T, dtype=jnp.int32).reshape(nqc, SEL_Q_CHUNK)
    b_ix = jnp.arange(B)[:, None, None, None]
    g_ix = jnp.arange(G)[None, None, :, None]
    offs = jnp.arange(SEL_BLOCK, dtype=jnp.int32)

    def one_chunk(args):
        qc, ic, tc = args
        kg = kb[b_ix, g_ix, ic]
        vg = vb[b_ix, g_ix, ic]
        kpos = ic[..., None] * SEL_BLOCK + offs
        mask = kpos <= tc[None, :, None, None, None]
        s = jnp.einsum('bcghd,bcgnld->bcghnl', qc, kg)
        s = s.reshape(B, SEL_Q_CHUNK, G, HG, n * SEL_BLOCK)
        p = masked_softmax(s, mask.reshape(B, SEL_Q_CHUNK, G, 1, n * SEL_BLOCK))
        return jnp.einsum('bcghk,bcgkd->bcghd', p.astype(vg.dtype),
                          vg.reshape(B, SEL_Q_CHUNK, G, n * SEL_BLOCK, DH))

    o = lax.map(one_chunk, (q_c, i_c, t_c))
    return jnp.moveaxis(o, 0, 1).reshape(B, T, G, HG, DH)


def window_attention(q, k_win, v_win):
    B, T, G, HG, DH = q.shape
    nq = T // WIN_Q_BLOCK
    r = WIN_SIZE // WIN_Q_BLOCK
    nk = (r + 1) * WIN_Q_BLOCK

    def band(u):
        up = jnp.pad(u, ((0, 0), (WIN_SIZE, 0), (0, 0), (0, 0)))
        up = up.reshape(B, (T + WIN_SIZE) // WIN_Q_BLOCK, WIN_Q_BLOCK, G, DH)
        return jnp.concatenate([up[:, i:i + nq] for i in range(r + 1)], axis=2)

    kw, vw = band(k_win), band(v_win)
    qb = q.reshape(B, nq, WIN_Q_BLOCK, G, HG, DH)
    a = jnp.arange(WIN_Q_BLOCK)[:, None]
    c = jnp.arange(nk)[None, :]
    rel = a - c + WIN_SIZE
    kpos = jnp.arange(nq)[:, None, None] * WIN_Q_BLOCK - WIN_SIZE + c[None]
    mask = (rel >= 0) & (rel < WIN_SIZE) & (kpos >= 0)
    s = jnp.einsum('bnqghd,bnkgd->bnghqk', qb, kw)
    p = masked_softmax(s, mask[None, :, None, None, :, :])
    o = jnp.einsum('bnghqk,bnkgd->bnqghd', p.astype(vw.dtype), vw)
    return o.reshape(B, T, G, HG, DH)


def nsa_mixer(q, k_cmp, v_cmp, k_sel, v_sel, k_win, v_win, g_br,
              cmp_pe, cmp_w1, cmp_b1, cmp_w2, cmp_b2):
    B, T = q.shape[:2]
    G, HG, DH = A_KV_GROUPS, A_HEADS_PER_GROUP, A_HEAD_DIM
    q = q.reshape(B, T, G, HG, DH) * (DH ** -0.5)
    kv = lambda u: u.reshape(B, T, G, DH)
    pos = jnp.arange(T, dtype=jnp.int32)
    kc = compress_tokens(kv(k_cmp), cmp_pe[0], cmp_w1[0], cmp_b1[0], cmp_w2[0], cmp_b2[0])
    vc = compress_tokens(kv(v_cmp), cmp_pe[1], cmp_w1[1], cmp_b1[1], cmp_w2[1], cmp_b2[1])
    nc = kc.shape[1]
    c_start = jnp.arange(nc, dtype=jnp.int32) * CMP_STRIDE
    c_mask = (c_start + CMP_BLOCK - 1)[None, :] <= pos[:, None]
    s_c = jnp.einsum('btghd,bngd->btghn', q, kc)
    p_c = masked_softmax(s_c, c_mask[None, :, None, None, :])
    o_cmp = jnp.einsum('btghn,bngd->btghd', p_c.astype(vc.dtype), vc)
    nb = T // SEL_BLOCK
    j = jnp.arange(nb, dtype=jnp.int32)
    overlap = ((c_start[:, None] <= j[None, :] * SEL_BLOCK + SEL_BLOCK - 1) &
               (c_start[:, None] + CMP_BLOCK - 1 >= j[None, :] * SEL_BLOCK)).astype(jnp.float32)
    p_slc = jnp.einsum('btgn,nj->btgj', jnp.sum(p_c, axis=3), overlap)
    cur = pos[:, None] // SEL_BLOCK
    valid = j[None, :] * SEL_BLOCK <= pos[:, None]
    forced = (j[None, :] == 0) | (j[None, :] == cur) | (j[None, :] == cur - 1)
    score = jnp.where(forced[None, :, None, :], jnp.inf,
                      jnp.where(valid[None, :, None, :], p_slc, -jnp.inf))
    n_sel = min(SEL_TOP_N, nb)
    _, idx = lax.top_k(score, n_sel)
    o_sel = selected_attention(q, kv(k_sel), kv(v_sel), idx.astype(jnp.int32))
    o_win = window_attention(q, kv(k_win), kv(v_win))
    g = jax.nn.sigmoid(g_br).reshape(B, T, G, HG, A_N_BRANCH)
    o = g[..., 0:1] * o_cmp + g[..., 1:2] * o_sel + g[..., 2:3] * o_win
    return o.reshape(B, T, A_WIDTH)


def gla_mixer(q, k, v, a_lr, alpha_w, alpha_b, norm_w):
    B, T = q.shape[:2]
    H, DK, DV, C = B_HEADS, B_KEY_DIM, B_VAL_DIM, B_CHUNK
    nck = T // C
    f32 = jnp.float32
    log_a = jax.nn.log_sigmoid((a_lr @ alpha_w + alpha_b).astype(f32)) / B_GATE_TEMP
    chunks = lambda u, d: u.astype(f32).reshape(B, nck, C, H, d)
    qc = chunks(q, DK) * (DK ** -0.5)
    kc = chunks(k, DK)
    vc = chunks(v, DV)
    cum = jnp.cumsum(chunks(log_a, DK), axis=2)
    last = cum[:, :, -1:]
    q_dec = qc * jnp.exp(cum)
    k_inv = kc * jnp.exp(-cum)
    k_state = kc * jnp.exp(last - cum)
    tril = jnp.tril(jnp.ones((C, C), dtype=bool))
    att = jnp.where(tril, jnp.einsum('bnthd,bnshd->bnhts', q_dec, k_inv), 0.0)
    o_intra = jnp.einsum('bnhts,bnshv->bnthv', att, vc)

    def step(S, xs):
        q_c, k_c, v_c, dec = xs
        o = jnp.einsum('bthd,bhdv->bthv', q_c, S)
        S = S * dec[..., None] + jnp.einsum('bshd,bshv->bhdv', k_c, v_c)
        return S, o

    S0 = jnp.zeros((B, H, DK, DV), f32)
    xs = (jnp.moveaxis(q_dec, 1, 0), jnp.moveaxis(k_state, 1, 0), jnp.moveaxis(vc, 1, 0),
          jnp.moveaxis(jnp.exp(last[:, :, 0]), 1, 0))
    _, o_inter = lax.scan(step, S0, xs)
    o = (o_intra + jnp.moveaxis(o_inter, 0, 1)).reshape(B, T, H, DV)
    o = o * lax.rsqrt(jnp.mean(o * o, axis=-1, keepdims=True) + NORM_EPS) * norm_w.astype(f32)
    return o.reshape(B, T, B_VAL_WIDTH).astype(q.dtype)


def setup_inputs(seed: int = 0) -> dict:
    key = jax.random.key(seed)
    ks = jax.random.split(key, 18)
    f32 = jnp.float32
    nrm = lambda k, shape, scale: jax.random.normal(k, shape, f32) * scale
    return {
        'x': nrm(ks[0], (BATCH, SEQ, D_MODEL), 1.0),
        'norm_w': 1.0 + nrm(ks[1], (DEPTH, D_MODEL), 0.02),
        'w_in': nrm(ks[2], (DEPTH, D_MODEL, IN_COLS), D_MODEL ** -0.5),
        'cmp_pe': nrm(ks[3], (DEPTH, 2, CMP_BLOCK, A_HEAD_DIM), 0.5),
        'cmp_w1': nrm(ks[4], (DEPTH, 2, CMP_BLOCK * A_HEAD_DIM, CMP_HIDDEN), (CMP_BLOCK * A_HEAD_DIM) ** -0.5),
        'cmp_b1': nrm(ks[5], (DEPTH, 2, CMP_HIDDEN), 0.01),
        'cmp_w2': nrm(ks[6], (DEPTH, 2, CMP_HIDDEN, A_HEAD_DIM), CMP_HIDDEN ** -0.5),
        'cmp_b2': nrm(ks[7], (DEPTH, 2, A_HEAD_DIM), 0.01),
        'gla_alpha_w': nrm(ks[8], (DEPTH, B_ALPHA_RANK, B_KEY_WIDTH), B_ALPHA_RANK ** -0.5),
        'gla_alpha_b': nrm(ks[9], (DEPTH, B_KEY_WIDTH), 0.1),
        'gla_norm_w': 1.0 + nrm(ks[10], (DEPTH, B_VAL_DIM), 0.02),
        'p_a': nrm(ks[11], (DEPTH, A_WIDTH, D_MODEL), A_WIDTH ** -0.5),
        'p_b': nrm(ks[12], (DEPTH, B_VAL_WIDTH, D_MODEL), B_VAL_WIDTH ** -0.5),
        'w_out': nrm(ks[13], (DEPTH, D_MODEL, D_MODEL), D_MODEL ** -0.5),
        'final_norm_w': 1.0 + nrm(ks[14], (D_MODEL,), 0.02),
    }


def reference(x, norm_w, w_in, cmp_pe, cmp_w1, cmp_b1, cmp_w2, cmp_b2,
              gla_alpha_w, gla_alpha_b, gla_norm_w, p_a, p_b, w_out, final_norm_w):
    split_points = np.cumsum(IN_SIZES)[:-1].tolist()
    for layer in range(DEPTH):
        h = rms_norm(x, norm_w[layer])
        cols = h @ w_in[layer]
        (q_a, k_cmp, v_cmp, k_sel, v_sel, k_win, v_win, g_br, z_a,
         q_b, k_b, v_b, a_lr, z_b, gate_a, gate_b) = jnp.split(cols, split_points, axis=-1)
        o_a = nsa_mixer(q_a, k_cmp, v_cmp, k_sel, v_sel, k_win, v_win, g_br,
                        cmp_pe[layer], cmp_w1[layer], cmp_b1[layer], cmp_w2[layer], cmp_b2[layer])
        y_a = (o_a * jax.nn.silu(z_a)) @ p_a[layer]
        o_b = gla_mixer(q_b, k_b, v_b, a_lr, gla_alpha_w[layer], gla_alpha_b[layer], gla_norm_w[layer])
        y_b = (o_b * jax.nn.silu(z_b)) @ p_b[layer]
        y = jax.nn.sigmoid(gate_a) * y_a + jax.nn.sigmoid(gate_b) * y_b
        x = x + y @ w_out[layer]
    return rms_norm(x, final_norm_w)
```

```python
import numpy as np
from contextlib import ExitStack
import concourse.bass as bass
import concourse.mybir as mybir
from concourse.bass_utils import run_bass_kernel_spmd
import ml_dtypes

F32 = mybir.dt.float32
BF16 = mybir.dt.bfloat16
AF = mybir.ActivationFunctionType
ALU = mybir.AluOpType
NP_BF16 = ml_dtypes.bfloat16

NCORES = 8
D = 2048
T = 8192
DEPTH = 4
IN_COLS = 17472
EPS = 1e-6
NEG = -30000.0

ENGS = ('pe', 'act', 'dve', 'pool', 'sp')


class LT:
    __slots__ = ('name', 'w', 'r', 'dkey', 'dcnt')

    def __init__(self, name):
        self.name = name
        self.w = None
        self.r = {}
        self.dkey = None
        self.dcnt = 0


class Sched:
    def __init__(self, nc, stack):
        self.nc = nc
        self.stack = stack
        self.ops = {e: [] for e in ENGS}
        self.cnt = {e: 0 for e in ENGS}
        self.waited = {e: {} for e in ENGS}
        self.sems = {}
        for e in ('pe', 'act', 'dve', 'pool'):
            self.sems[e] = stack.enter_context(nc.semaphore('s_' + e))
        self.nlt = 0
        self.dma_lts = []
        self.same_engine_sync = True

    def lt(self, name, dma=False):
        t = LT(name)
        if dma:
            key = 'd%d_%s' % (self.nlt, name)
            self.nlt += 1
            self.sems[key] = self.stack.enter_context(self.nc.semaphore(key))
            t.dkey = key
            self.dma_lts.append(t)
        return t

    def _deps(self, eng, reads, writes):
        deps = {}

        def add(k, v):
            if deps.get(k, 0) < v:
                deps[k] = v
        for t in reads:
            if t.w:
                add(*t.w)
        for t in writes:
            if t.w:
                add(*t.w)
            for k, v in t.r.items():
                add(k, v)
        waits = []
        for k, v in deps.items():
            if k == eng and (eng == 'pe' or not self.same_engine_sync):
                continue
            if self.waited[eng].get(k, 0) >= v:
                continue
            self.waited[eng][k] = v
            waits.append((k, v))
        return waits

    def op(self, eng, fn, reads=(), writes=()):
        waits = self._deps(eng, reads, writes)
        self.cnt[eng] += 1
        c = self.cnt[eng]
        self.ops[eng].append((waits, fn, (eng, 1)))
        for t in writes:
            t.w = (eng, c)
            t.r = {}
        for t in reads:
            if t.r.get(eng, 0) < c:
                t.r[eng] = c

    def dma(self, q, out_ap, in_ap, sem_lt, reads=(), writes=()):
        waits = self._deps(q, reads, writes)
        sem_lt.dcnt += 16
        v = sem_lt.dcnt
        key = sem_lt.dkey

        def fn(e, out_ap=out_ap, in_ap=in_ap):
            return e.dma_start(out=out_ap, in_=in_ap)
        self.ops[q].append((waits, fn, (key, 16)))
        for t in writes:
            t.w = (key, v)
            t.r = {}
        for t in reads:
            if t.r.get(key, 0) < v:
                t.r[key] = v

    def finish(self):
        waits = []
        for t in self.dma_lts:
            if t.dcnt > 0:
                waits.append((t.dkey, t.dcnt))
        for e in ('pe', 'act', 'dve', 'pool'):
            if self.cnt[e] > 0:
                waits.append((e, self.cnt[e]))
        self.ops['sp'].append((waits, None, None))

    def emit(self):
        nc = self.nc
        sems = self.sems
        ops = self.ops

        def mk(e):
            def body(eng):
                for waits, fn, inc in ops[e]:
                    for k, v in waits:
                        eng.wait_ge(sems[k], v)
                    if fn is None:
                        continue
                    ins = fn(eng)
                    if inc is not None:
                        ins.then_inc(sems[inc[0]], inc[1])
            return body
        with nc.Block() as block:
            block.tensor(mk('pe'))
            block.scalar(mk('act'))
            block.vector(mk('dve'))
            block.gpsimd(mk('pool'))
            block.sync(mk('sp'))


class Ctx:
    def __init__(self):
        self.nc = bass.Bass("TRN2", target_bir_lowering=False)
        self.stack = ExitStack()
        self.S = Sched(self.nc, self.stack)
        self.ninputs = []
        self.outputs = []

    def din(self, name, shape, dt):
        self.ninputs.append(name)
        return self.nc.dram_tensor(name, list(shape), dt, kind="ExternalInput").ap()

    def dout(self, name, shape, dt):
        self.outputs.append(name)
        return self.nc.dram_tensor(name, list(shape), dt, kind="ExternalOutput").ap()

    def sb(self, name, shape, dt):
        return self.stack.enter_context(self.nc.sbuf_tensor('sb_' + name, list(shape), dt))

    def ps(self, name, shape=(128, 512), dt=F32):
        return self.stack.enter_context(self.nc.psum_tensor('pp_' + name, list(shape), dt))

    def done(self):
        self.S.finish()
        self.S.emit()
        self.stack.close()
        return self.nc


def mm(S, ps_lt, out_ap, lhsT, rhs, start, stop, reads):
    def fn(e):
        return e.matmul(out_ap, lhsT, rhs, start=start, stop=stop)
    S.op('pe', fn, reads=reads, writes=[ps_lt])


def act(S, out_ap, in_ap, func, reads, writes, scale=1.0, bias=0.0, eng='act'):
    def fn(e):
        return e.activation(out_ap, in_ap, func, bias=bias, scale=scale)
    S.op('act', fn, reads=reads, writes=writes)


TPC = T // NCORES

L1_SPECS = [
    ('qaT', 0, 2048, 'F', 'copy', 128 ** -0.5, BF16),
    ('kcmpT', 2048, 512, 'F', 'copy', 1.0, BF16),
    ('vcmpT', 2560, 512, 'F', 'copy', 1.0, BF16),
    ('kselT', 3072, 512, 'F', 'copy', 1.0, BF16),
    ('vsel', 3584, 512, 'T', 'copy', 1.0, BF16),
    ('kwinT', 4096, 512, 'F', 'copy', 1.0, BF16),
    ('vwin', 4608, 512, 'T', 'copy', 1.0, BF16),
    ('gbrT', 5120, 48, 'F', 'sigmoid', 1.0, F32),
    ('szaT', 5168, 2048, 'F', 'silu', 1.0, BF16),
    ('qbT', 7216, 1024, 'F', 'copy', 256 ** -0.5, BF16),
    ('kbT', 8240, 1024, 'F', 'copy', 1.0, BF16),
    ('kb', 8240, 1024, 'T', 'copy', 1.0, BF16),
    ('vb', 9264, 2048, 'T', 'copy', 1.0, BF16),
    ('alrT', 11312, 16, 'F', 'copy', 1.0, F32),
    ('szbT', 11328, 2048, 'F', 'silu', 1.0, BF16),
    ('sgaT', 13376, 2048, 'F', 'sigmoid', 1.0, BF16),
    ('sgbT', 15424, 2048, 'F', 'sigmoid', 1.0, BF16),
]
FUNCS = {'copy': AF.Copy, 'sigmoid': AF.Sigmoid, 'silu': AF.Silu}


def np_dt(dt):
    return np.float32 if dt == F32 else NP_BF16


def proj_slabs(C, hT, hT_lt, w_ap, slabs, KC, NT, wst, wst_lt, wbf, wbf_lt, psb, ost, ost_lt, ostf, ostf_lt,
               evac=None):
    S = C.S
    pi = 0
    hT0, hT_lt0, w_ap0 = hT, hT_lt, w_ap
    for si, (c0, w, layout, func, scale, out_ap, dt, extra) in enumerate(slabs):
        b = si % 2
        hT, hT_lt = extra.get('hT', (hT0, hT_lt0))
        w_ap = extra.get('w', w_ap0)
        evac = extra.get('evac', None)
        if extra.get('prefn') is not None:
            extra['prefn'](extra)
        wv = w_ap[:, c0:c0 + w].rearrange("(kc p) n -> p kc n", p=128)
        kq = max(1, KC // 4)
        for j in range(0, KC, kq):
            S.dma('sp', wst[b][:, j:j + kq, 0:w], wv[:, j:j + kq, :], wst_lt[b], writes=[wst_lt[b]])
        for jj, j in enumerate(range(0, KC, kq)):
            e = 'pool' if jj % 2 == 0 else 'dve'
            S.op(e, (lambda en, b=b, j=j, w=w, kq=kq: en.tensor_copy(wbf[b][:, j:j + kq, 0:w], wst[b][:, j:j + kq, 0:w])),
                 reads=[wst_lt[b]], writes=[wbf_lt[b]])
        isf32 = (dt == F32)
        if ost is None:
            o_t, o_lt = None, None
        else:
            o_t, o_lt = (ostf, ostf_lt) if isf32 else (ost[b], ost_lt[b])
        if layout == 'F':
            nch = (w + 127) // 128
            TT = 512
            for ncx in range(nch):
                m = min(128, w - ncx * 128)
                for tt in range(NT // TT):
                    pst, pslt = psb[pi % len(psb)]
                    pi += 1
                    for k in range(KC):
                        mm(S, pslt, pst[0:m, :], wbf[b][:, k, ncx * 128:ncx * 128 + m], hT[:, k, tt * TT:(tt + 1) * TT],
                           k == 0, k == KC - 1, [wbf_lt[b], hT_lt])
                    dst = o_t[0:m, ncx * NT + tt * TT: ncx * NT + (tt + 1) * TT] if o_t is not None else None
                    if evac is not None:
                        evac(extra, ncx, tt, TT, m, pst, pslt, dst, o_lt)
                    else:
                        act(S, dst, pst[0:m, :], FUNCS[func], [pslt], [o_lt], scale=scale)
            for ncx in range(nch if not extra.get('nostore') else 0):
                m = min(128, w - ncx * 128)
                S.dma('sp', out_ap[c0_out(extra, c0) + ncx * 128: c0_out(extra, c0) + ncx * 128 + m, :],
                      o_t[0:m, ncx * NT:(ncx + 1) * NT], o_lt, reads=[o_lt])
        else:
            assert w == 512
            for tt in range(NT // 128):
                pst, pslt = psb[pi % len(psb)]
                pi += 1
                for k in range(KC):
                    mm(S, pslt, pst[:, :], hT[:, k, tt * 128:(tt + 1) * 128], wbf[b][:, k, 0:512],
                       k == 0, k == KC - 1, [wbf_lt[b], hT_lt])
                dst = o_t[:, tt * 512:(tt + 1) * 512]
                if tt % 2 == 0:
                    act(S, dst, pst[:, :], AF.Copy, [pslt], [o_lt], scale=scale)
                else:
                    S.op('dve', (lambda en, dst=dst, pst=pst: en.tensor_copy(dst, pst[:, :])), reads=[pslt], writes=[o_lt])
            oc0 = c0_out(extra, c0)
            S.dma('sp', out_ap[:, oc0:oc0 + 512].rearrange("(c p) n -> p c n", p=128),
                  o_t[:, 0:(NT // 128) * 512].rearrange("p (c n) -> p c n", n=512), o_lt, reads=[o_lt])


def c0_out(extra, c0):
    return extra['o0']


def build_l1():
    C = Ctx()
    nc, S = C.nc, C.S
    xT = C.din('xT', (D, TPC), F32)
    nw = C.din('nw', (128, 16), F32)
    w_in = C.din('w_in', (D, IN_COLS), F32)
    outs = {}
    for (name, c0, ncols, layout, func, scale, dt) in L1_SPECS:
        shape = (ncols, TPC) if layout == 'F' else (TPC, ncols)
        outs[name] = C.dout(name, shape, dt)

    KC = 16
    xs = C.sb('xs', (128, KC, 512), F32)
    xs_lt = S.lt('xs', dma=True)
    hT = C.sb('hT', (128, KC, TPC), BF16)
    hT_lt = S.lt('hT')
    nws = C.sb('nws', (128, 16), F32)
    nws_lt = S.lt('nws', dma=True)
    ones = C.sb('ones', (128, 128), F32)
    ones_lt = S.lt('ones')
    sq = [C.sb('sq%d' % i, (128, 512), F32) for i in range(2)]
    sq_lt = [S.lt('sq%d' % i) for i in range(2)]
    rstd = C.sb('rstd', (128, 512), F32)
    rstd_lt = S.lt('rstd')
    wst = [C.sb('wst%d' % i, (128, KC, 512), F32) for i in range(2)]
    wst_lt = [S.lt('wst%d' % i, dma=True) for i in range(2)]
    wbf = [C.sb('wbf%d' % i, (128, KC, 512), BF16) for i in range(2)]
    wbf_lt = [S.lt('wbf%d' % i) for i in range(2)]
    ost = [C.sb('ost%d' % i, (128, 4096), BF16) for i in range(2)]
    ost_lt = [S.lt('ost%d' % i, dma=True) for i in range(2)]
    ostf = C.sb('ostf', (128, 1024), F32)
    ostf_lt = S.lt('ostf', dma=True)
    psb = []
    for i in range(8):
        psb.append((C.ps('ps%d' % i), S.lt('ps%d' % i)))

    S.dma('sp', nws[:, :], nw[:, :], nws_lt, writes=[nws_lt])
    S.op('dve', lambda e: e.memset(ones[:, :], 1.0), writes=[ones_lt])

    xv = xT.rearrange("(kc p) t -> p kc t", p=128)
    for th in range(TPC // 512):
        for j in range(0, KC, 4):
            S.dma('sp', xs[:, j:j + 4, :], xv[:, j:j + 4, th * 512:(th + 1) * 512], xs_lt, writes=[xs_lt])
        pst, pslt = psb[th % 8]
        for k in range(KC):
            i = k % 2
            act(S, sq[i][:, :], xs[:, k, :], AF.Square, [xs_lt], [sq_lt[i]])
            mm(S, pslt, pst[:, :], ones[:, :], sq[i][:, :], k == 0, k == KC - 1, [ones_lt, sq_lt[i]])
        S.op('dve', lambda e, pst=pst: e.tensor_scalar(rstd[:, :], pst[:, :], 1.0 / D, EPS, ALU.mult, ALU.add),
             reads=[pslt], writes=[rstd_lt])
        act(S, rstd[:, :], rstd[:, :], AF.Sqrt, [rstd_lt], [rstd_lt])
        S.op('dve', lambda e: e.reciprocal(rstd[:, :], rstd[:, :]), reads=[rstd_lt], writes=[rstd_lt])
        for k in range(KC):
            e = 'dve'
            S.op(e, (lambda en, k=k, th=th: en.scalar_tensor_tensor(
                hT[:, k, th * 512:(th + 1) * 512], xs[:, k, :], nws[:, k:k + 1], rstd[:, :], ALU.mult, ALU.mult)),
                reads=[xs_lt, nws_lt, rstd_lt], writes=[hT_lt])

    slabs = []
    for (name, c0, ncols, layout, func, scale, dt) in L1_SPECS:
        for s0 in range(0, ncols, 512):
            w = min(512, ncols - s0)
            slabs.append((c0 + s0, w, layout, func, scale, outs[name], dt, {'o0': s0}))
    proj_slabs(C, hT, hT_lt, w_in, slabs, KC, TPC, wst, wst_lt, wbf, wbf_lt, psb, ost, ost_lt, ostf, ostf_lt)
    return C.done(), C


NQT = 32
NEGB = -32768.0


def tt(S, eng, out, a, b, op, reads, writes):
    S.op(eng, lambda e: e.tensor_tensor(out, a, b, op), reads=reads, writes=writes)


def ts(S, eng, out, a, s1, s2, op0, op1, reads, writes):
    if s2 is None:
        S.op(eng, lambda e: e.tensor_scalar(out, a, s1, None, op0), reads=reads, writes=writes)
    else:
        S.op(eng, lambda e: e.tensor_scalar(out, a, s1, s2, op0, op1), reads=reads, writes=writes)


def build_nsa(qts=None):
    C = Ctx()
    nc, S = C.nc, C.S
    qs_d = C.din('qs', (128, NQT * 512), BF16)
    sza_d = C.din('sza', (128, NQT * 512), BF16)
    gs_d = C.din('gs', (12, NQT * 128), F32)
    addm_d = C.din('addm', (128, NQT * 128), F32)
    kselT_d = C.din('kselT', (128, T), BF16)
    vsel_d = C.din('vsel', (128, T), BF16)
    kwinT_d = C.din('kwinT', (128, T), BF16)
    vwin_d = C.din('vwin', (128, T), BF16)
    kcmpT_d = C.din('kcmpT', (128, T), BF16)
    vcmpT_d = C.din('vcmpT', (128, T), BF16)
    w1_d = C.din('w1', (2, 128, 32 * 256), F32)
    peT_d = C.din('peT', (128, 64), F32)
    b1_d = C.din('b1', (128, 4), F32)
    w2_d = C.din('w2', (128, 2 * 2 * 128), F32)
    b2k_d = C.din('b2k', (128, 1), F32)
    b2v_d = C.din('b2v', (1, 128), F32)
    identb_d = C.din('identb', (128, 128), BF16)
    identf_d = C.din('identf', (128, 128), F32)
    eexp_d = C.din('eexp', (128, T), BF16)
    dmask_d = C.din('dmask', (128, 6 * 128), BF16)
    cmask_d = C.din('cmask', (128, 9 * 128), BF16)
    ovl_d = C.din('ovl', (128, 4 * 128), BF16)
    onehot_d = C.din('onehot', (12, 12 * 128), F32)
    ua_d = C.dout('ua', (128, NQT * 512), BF16)

    def ld(name, src, shape, dt):
        t = C.sb(name, shape, dt)
        l = S.lt(name, dma=True)
        fs = shape[1]
        step = 2048
        for o in range(0, fs, step):
            e = min(fs, o + step)
            S.dma('sp', t[:, o:e], src[:, o:e], l, writes=[l])
        return t, l

    kselT, kselT_lt = ld('kselT', kselT_d, (128, T), BF16)
    vsel, vsel_lt = ld('vsel', vsel_d, (128, T), BF16)
    kwinT, kwinT_lt = ld('kwinT', kwinT_d, (128, T), BF16)
    vwin, vwin_lt = ld('vwin', vwin_d, (128, T), BF16)
    eexp, eexp_lt = ld('eexp', eexp_d, (128, T), BF16)
    identb, identb_lt = ld('identb', identb_d, (128, 128), BF16)
    identf, identf_lt = ld('identf', identf_d, (128, 128), F32)
    dmask, dmask_lt = ld('dmask', dmask_d, (128, 6 * 128), BF16)
    cmask, cmask_lt = ld('cmask', cmask_d, (128, 9 * 128), BF16)
    ovl, ovl_lt = ld('ovl', ovl_d, (128, 4 * 128), BF16)
    onehot, onehot_lt = ld('onehot', onehot_d, (12, 12 * 128), F32)
    peT, peT_lt = ld('peT', peT_d, (128, 64), F32)
    b1, b1_lt = ld('b1', b1_d, (128, 4), F32)
    w2f, w2f_lt = ld('w2f', w2_d, (128, 512), F32)
    b2k, b2k_lt = ld('b2k', b2k_d, (128, 1), F32)
    b2vf, b2vf_lt = ld('b2vf', b2v_d, (1, 128), F32)

    onesb = C.sb('onesb', (128, 128), BF16)
    onesb_lt = S.lt('onesb')
    S.op('dve', lambda e: e.memset(onesb[:, :], 1.0), writes=[onesb_lt])
    w2b = C.sb('w2b', (128, 512), BF16)
    w2b_lt = S.lt('w2b')
    S.op('dve', lambda e: e.tensor_copy(w2b[:, :], w2f[:, :]), reads=[w2f_lt], writes=[w2b_lt])
    b2vb = C.sb('b2vb', (1, 128), BF16)
    b2vb_lt = S.lt('b2vb')
    S.op('dve', lambda e: e.tensor_copy(b2vb[:, :], b2vf[:, :]), reads=[b2vf_lt], writes=[b2vb_lt])
    peb = C.sb('peb', (128, 64), BF16)
    peb_lt = S.lt('peb')
    S.op('dve', lambda e: e.tensor_copy(peb[:, :], peT[:, :]), reads=[peT_lt], writes=[peb_lt])

    bank = [(C.ps('bk%d' % i), S.lt('bk%d' % i)) for i in range(8)]
    SB = bank[0:3]
    OA, DA, OW, DW, MI = bank[3], bank[4], bank[5], bank[6], bank[7]

    cin = C.sb('cin', (128, T), BF16)
    cin_lt = S.lt('cin', dma=True)
    w1s = C.sb('w1s', (128, 8 * 256), F32)
    w1s_lt = S.lt('w1s', dma=True)
    w1b = C.sb('w1b', (128, 32 * 256), BF16)
    w1b_lt = S.lt('w1b')
    kcT = C.sb('kcT', (128, 512), BF16)
    kcT_lt = S.lt('kcT')
    vc = C.sb('vc', (128, 512), BF16)
    vc_lt = S.lt('vc')
    hidT = C.sb('hidT', (128, 2 * 512), BF16)
    hidT_lt = S.lt('hidT')
    bt = C.sb('bt', (128, 2), F32)
    bt_lt = S.lt('bt')
    g_u = C.sb('g_u', (128, 512), F32)
    g_u_lt = S.lt('g_u')
    g_a = C.sb('g_a', (128, 512), F32)
    g_a_lt = S.lt('g_a')
    g_s = C.sb('g_s', (128, 512), F32)
    g_s_lt = S.lt('g_s')
    S.op('dve', lambda e: e.memset(hidT[:, :], 0.0), writes=[hidT_lt])
    for kv in range(2):
        src = kcmpT_d if kv == 0 else vcmpT_d
        for o in range(0, T, 2048):
            S.dma('sp', cin[:, o:o + 2048], src[:, o:o + 2048], cin_lt, writes=[cin_lt])
        for lq in range(4):
            S.dma('sp', w1s[:, :], w1_d[kv, :, lq * 2048:(lq + 1) * 2048], w1s_lt, writes=[w1s_lt])
            S.op('pool', (lambda e, lq=lq: e.tensor_copy(w1b[:, lq * 2048:(lq + 1) * 2048], w1s[:, :])),
                 reads=[w1s_lt], writes=[w1b_lt])
        cv = cin.rearrange("p (n s) -> p n s", s=16)
        for hc in range(2):
            pst, pslt = SB[hc]
            for l in range(32):
                rhs = cv[:, 0:511, l] if l < 16 else cv[:, 1:512, l - 16]
                mm(S, pslt, pst[:, 0:511], w1b[:, l * 256 + hc * 128: l * 256 + hc * 128 + 128], rhs,
                   l == 0, l == 31, [w1b_lt, cin_lt])
            pm, pmlt = MI
            for l in range(32):
                mm(S, pmlt, pm[:, 0:1], w1b[:, l * 256 + hc * 128: l * 256 + hc * 128 + 128],
                   peb[:, kv * 32 + l: kv * 32 + l + 1], l == 0, l == 31, [w1b_lt, peb_lt])
            tt(S, 'dve', bt[:, hc:hc + 1], pm[:, 0:1], b1[:, kv * 2 + hc: kv * 2 + hc + 1], ALU.add,
               [pmlt, b1_lt], [bt_lt])
            ts(S, 'dve', g_u[:, 0:511], pst[:, 0:511], bt[:, hc:hc + 1], None, ALU.add, None, [pslt, bt_lt], [g_u_lt])
            tt(S, 'dve', g_a[:, 0:511], g_u[:, 0:511], g_u[:, 0:511], ALU.mult, [g_u_lt], [g_a_lt])
            ts(S, 'dve', g_a[:, 0:511], g_a[:, 0:511], 0.044715, 1.0, ALU.mult, ALU.add, [g_a_lt], [g_a_lt])
            tt(S, 'dve', g_a[:, 0:511], g_a[:, 0:511], g_u[:, 0:511], ALU.mult, [g_a_lt, g_u_lt], [g_a_lt])
            act(S, g_s[:, 0:511], g_a[:, 0:511], AF.Sigmoid, [g_a_lt], [g_s_lt], scale=1.5957691216057308)
            tt(S, 'dve', hidT[:, hc * 512: hc * 512 + 511], g_u[:, 0:511], g_s[:, 0:511], ALU.mult,
               [g_u_lt, g_s_lt], [hidT_lt])
        if kv == 0:
            pst, pslt = SB[2]
            for hc in range(2):
                mm(S, pslt, pst[:, 0:512], w2b[:, (0 * 2 + hc) * 128:(0 * 2 + hc) * 128 + 128],
                   hidT[:, hc * 512:(hc + 1) * 512], hc == 0, hc == 1, [w2b_lt, hidT_lt])
            ts(S, 'dve', kcT[:, :], pst[:, :], b2k[:, 0:1], None, ALU.add, None, [pslt, b2k_lt], [kcT_lt])
        else:
            pst, pslt = SB[2]
            for nt in range(4):
                for hc in range(2):
                    mm(S, pslt, pst[:, nt * 128:(nt + 1) * 128], hidT[:, hc * 512 + nt * 128: hc * 512 + nt * 128 + 128],
                       w2b[:, (1 * 2 + hc) * 128:(1 * 2 + hc) * 128 + 128], hc == 0, False, [w2b_lt, hidT_lt])
                mm(S, pslt, pst[:, nt * 128:(nt + 1) * 128], onesb[0:1, :], b2vb[0:1, :], False, True,
                   [onesb_lt, b2vb_lt])
            S.op('dve', lambda e, pst=pst: e.tensor_copy(vc[:, :], pst[:, :]), reads=[pslt], writes=[vc_lt])

    qsb = [C.sb('qsb%d' % i, (128, 512), BF16) for i in range(2)]
    qsb_lt = [S.lt('qsb%d' % i, dma=True) for i in range(2)]
    szb = [C.sb('szb%d' % i, (128, 512), BF16) for i in range(2)]
    szb_lt = [S.lt('szb%d' % i, dma=True) for i in range(2)]
    gsb = [C.sb('gsb%d' % i, (12, 128), F32) for i in range(2)]
    gsb_lt = [S.lt('gsb%d' % i, dma=True) for i in range(2)]
    adb = [C.sb('adb%d' % i, (128, 128), F32) for i in range(2)]
    adb_lt = [S.lt('adb%d' % i, dma=True) for i in range(2)]
    uo = [C.sb('uo%d' % i, (128, 512), BF16) for i in range(2)]
    uo_lt = [S.lt('uo%d' % i, dma=True) for i in range(2)]
    Ec = C.sb('Ec', (128, 4 * 512), BF16)
    Ec_lt = [S.lt('Ec%d' % i) for i in range(4)]
    NP = 3
    Pb = [C.sb('Pb%d' % i, (128, 512), BF16) for i in range(NP)]
    Pb_lt = [S.lt('Pb%d' % i) for i in range(NP)]
    Rc = C.sb('Rc', (128, 512), F32)
    Rc_lt = S.lt('Rc')
    Rs = C.sb('Rs', (128, 512), F32)
    Rs_lt = S.lt('Rs')
    Rw = C.sb('Rw', (128, 512), F32)
    Rw_lt = S.lt('Rw')
    Gs = C.sb('Gs', (128, 3 * 512), F32)
    Gs_lt = [S.lt('Gs%d' % i) for i in range(3)]
    acc = C.sb('acc', (128, 512), F32)
    acc_lt = S.lt('acc')
    tmp = C.sb('tmp', (128, 512), F32)
    tmp_lt = S.lt('tmp')
    score = C.sb('score', (128, 128), F32)
    score_lt = S.lt('score')
    sc2 = C.sb('sc2', (128, 128), F32)
    sc2_lt = S.lt('sc2')
    m8 = C.sb('m8', (128, 16), F32)
    m8_lt = S.lt('m8')
    thr = C.sb('thr', (128, 1), F32)
    thr_lt = S.lt('thr')
    selm = C.sb('selm', (128, 128), F32)
    selm_lt = S.lt('selm')
    selT = C.sb('selT', (128, 128), BF16)
    selT_lt = S.lt('selT')
    si = [0]
    pi = [0]

    def bc4(ap128):
        return ap128.unsqueeze(1).broadcast_to([128, 4, 128])

    def sbank():
        b = SB[si[0] % 3]
        si[0] += 1
        return b

    def pbuf():
        i = pi[0] % NP
        pi[0] += 1
        return Pb[i], Pb_lt[i]

    for qidx, qt in enumerate(qts if qts is not None else range(NQT)):
        b = qidx % 2
        S.dma('sp', qsb[b][:, :], qs_d[:, qt * 512:(qt + 1) * 512], qsb_lt[b], writes=[qsb_lt[b]])
        S.dma('sp', szb[b][:, :], sza_d[:, qt * 512:(qt + 1) * 512], szb_lt[b], writes=[szb_lt[b]])
        S.dma('sp', gsb[b][:, :], gs_d[:, qt * 128:(qt + 1) * 128], gsb_lt[b], writes=[gsb_lt[b]])
        S.dma('sp', adb[b][:, :], addm_d[:, qt * 128:(qt + 1) * 128], adb_lt[b], writes=[adb_lt[b]])
        q = qsb[b]
        qlt = qsb_lt[b]
        nnt = (16 * qt + 6) // 128 + 1
        for nt in range(nnt):
            rr = qt - 8 * nt
            pst, pslt = sbank()
            partial = rr <= 8
            mm(S, pslt, pst[:, :], kcT[:, nt * 128:(nt + 1) * 128], q[:, :], True, not partial, [kcT_lt, qlt])
            if partial:
                mm(S, pslt, pst[:, :].rearrange("p (h q) -> p h q", h=4), identb[:, :],
                   bc4(cmask[:, rr * 128:(rr + 1) * 128]), False, True, [identb_lt, cmask_lt])
            act(S, Ec[:, nt * 512:(nt + 1) * 512], pst[:, :], AF.Exp, [pslt], [Ec_lt[nt]])
            mm(S, DA[1], DA[0][:, :], onesb[:, :], Ec[:, nt * 512:(nt + 1) * 512], nt == 0, nt == nnt - 1,
               [onesb_lt, Ec_lt[nt]])
        ts(S, 'dve', Rc[:, :], DA[0][:, :], 1e-30, None, ALU.max, None, [DA[1]], [Rc_lt])
        S.op('dve', lambda e: e.reciprocal(Rc[:, :], Rc[:, :]), reads=[Rc_lt], writes=[Rc_lt])
        for nt in range(nnt):
            tt(S, 'pool' if nt % 2 else 'dve', Ec[:, nt * 512:(nt + 1) * 512], Ec[:, nt * 512:(nt + 1) * 512], Rc[:, :],
               ALU.mult, [Ec_lt[nt], Rc_lt], [Ec_lt[nt]])
        for nt in range(nnt):
            mm(S, OA[1], OA[0][:, :], vc[:, nt * 128:(nt + 1) * 128], Ec[:, nt * 512:(nt + 1) * 512], nt == 0,
               nt == nnt - 1, [vc_lt, Ec_lt[nt]])
        k = 0
        for nt in range(nnt):
            for h in range(4):
                mm(S, MI[1], MI[0][:, 0:128], Ec[:, nt * 512 + h * 128: nt * 512 + (h + 1) * 128],
                   ovl[:, nt * 128:(nt + 1) * 128], k == 0, k == nnt * 4 - 1, [Ec_lt[nt], ovl_lt])
                k += 1
        tt(S, 'dve', score[:, :], MI[0][:, 0:128], adb[b][:, :], ALU.add, [MI[1], adb_lt[b]], [score_lt])
        S.op('dve', lambda e: e.max(out=m8[:, 0:8], in_=score[:, :]), reads=[score_lt], writes=[m8_lt])
        S.op('dve', lambda e: e.match_replace(out=sc2[:, :], in_to_replace=m8[:, 0:8], in_values=score[:, :],
                                              imm_value=-1e9), reads=[score_lt, m8_lt], writes=[sc2_lt])
        S.op('dve', lambda e: e.max(out=m8[:, 8:16], in_=sc2[:, :]), reads=[sc2_lt], writes=[m8_lt])
        S.op('dve', lambda e: e.tensor_reduce(thr[:, :], m8[:, 8:16], mybir.AxisListType.X, ALU.min),
             reads=[m8_lt], writes=[thr_lt])
        ts(S, 'dve', selm[:, :], score[:, :], thr[:, 0:1], -1.0, ALU.is_ge, ALU.add, [score_lt, thr_lt], [selm_lt])
        S.op('pe', lambda e: e.transpose(MI[0][:, 128:256], selm[:, :], identf[:, :]),
             reads=[selm_lt, identf_lt], writes=[MI[1]])
        S.op('dve', lambda e: e.tensor_copy(selT[:, :], MI[0][:, 128:256]), reads=[MI[1]], writes=[selT_lt])
        for br in range(3):
            for h in range(4):
                r = h * 3 + br
                mm(S, MI[1], MI[0][:, h * 128:(h + 1) * 128], onehot[0:12, r * 128:(r + 1) * 128], gsb[b][0:12, :],
                   True, True, [onehot_lt, gsb_lt[b]])
            act(S, Gs[:, br * 512:(br + 1) * 512], MI[0][:, :], AF.Copy, [MI[1]], [Gs_lt[br]])
        tt(S, 'dve', acc[:, :], OA[0][:, :], Gs[:, 0:512], ALU.mult, [OA[1], Gs_lt[0]], [acc_lt])
        nk = 2 * qt + 2
        for kt in range(nk):
            pst, pslt = sbank()
            mm(S, pslt, pst[:, :], kselT[:, kt * 128:(kt + 1) * 128], q[:, :], True, False, [kselT_lt, qlt])
            dm = kt - 2 * qt
            mm(S, pslt, pst[:, :].rearrange("p (h q) -> p h q", h=4), eexp[:, kt * 128:(kt + 1) * 128],
               bc4(selT[:, :]), False, dm < 0, [eexp_lt, selT_lt])
            if dm >= 0:
                mm(S, pslt, pst[:, :].rearrange("p (h q) -> p h q", h=4), identb[:, :],
                   bc4(dmask[:, dm * 128:(dm + 1) * 128]), False, True, [identb_lt, dmask_lt])
            P, Plt = pbuf()
            act(S, P[:, :], pst[:, :], AF.Exp, [pslt], [Plt])
            mm(S, OA[1], OA[0][:, :], vsel[:, kt * 128:(kt + 1) * 128], P[:, :], kt == 0, kt == nk - 1, [vsel_lt, Plt])
            mm(S, DA[1], DA[0][:, :], onesb[:, :], P[:, :], kt == 0, kt == nk - 1, [onesb_lt, Plt])
        kts = [kt for kt in range(2 * qt - 4, 2 * qt + 2) if kt >= 0]
        for ii, kt in enumerate(kts):
            pst, pslt = sbank()
            pos = kt - (2 * qt - 4)
            mi = {0: 2, 1: 3, 4: 4, 5: 5}.get(pos)
            mm(S, pslt, pst[:, :], kwinT[:, kt * 128:(kt + 1) * 128], q[:, :], True, mi is None, [kwinT_lt, qlt])
            if mi is not None:
                mm(S, pslt, pst[:, :].rearrange("p (h q) -> p h q", h=4), identb[:, :],
                   bc4(dmask[:, mi * 128:(mi + 1) * 128]), False, True, [identb_lt, dmask_lt])
            P, Plt = pbuf()
            act(S, P[:, :], pst[:, :], AF.Exp, [pslt], [Plt])
            mm(S, OW[1], OW[0][:, :], vwin[:, kt * 128:(kt + 1) * 128], P[:, :], ii == 0, ii == len(kts) - 1,
               [vwin_lt, Plt])
            mm(S, DW[1], DW[0][:, :], onesb[:, :], P[:, :], ii == 0, ii == len(kts) - 1, [onesb_lt, Plt])
        S.op('dve', lambda e: e.reciprocal(Rs[:, :], DA[0][:, :]), reads=[DA[1]], writes=[Rs_lt])
        tt(S, 'pool', Rs[:, :], Rs[:, :], Gs[:, 512:1024], ALU.mult, [Rs_lt, Gs_lt[1]], [Rs_lt])
        tt(S, 'dve', tmp[:, :], OA[0][:, :], Rs[:, :], ALU.mult, [OA[1], Rs_lt], [tmp_lt])
        tt(S, 'pool', acc[:, :], acc[:, :], tmp[:, :], ALU.add, [acc_lt, tmp_lt], [acc_lt])
        S.op('dve', lambda e: e.reciprocal(Rw[:, :], DW[0][:, :]), reads=[DW[1]], writes=[Rw_lt])
        tt(S, 'pool', Rw[:, :], Rw[:, :], Gs[:, 1024:1536], ALU.mult, [Rw_lt, Gs_lt[2]], [Rw_lt])
        tt(S, 'dve', tmp[:, :], OW[0][:, :], Rw[:, :], ALU.mult, [OW[1], Rw_lt], [tmp_lt])
        tt(S, 'pool', acc[:, :], acc[:, :], tmp[:, :], ALU.add, [acc_lt, tmp_lt], [acc_lt])
        tt(S, 'pool', uo[b][:, :], acc[:, :], szb[b][:, :], ALU.mult, [acc_lt, szb_lt[b]], [uo_lt[b]])
        S.dma('sp', ua_d[:, qt * 512:(qt + 1) * 512], uo[b][:, :], uo_lt[b], reads=[uo_lt[b]])
    return C.done(), C


def nsa_consts(par):
    k = np.arange(128)
    causal = np.where(k[:, None] > k[None, :], NEGB, 0.0).astype(np.float32)
    winm = np.where(k[:, None] <= k[None, :], NEGB, 0.0).astype(np.float32)
    alln = np.full((128, 128), NEGB, np.float32)
    zer = np.zeros((128, 128), np.float32)
    if par == 0:
        dm = [causal, alln, winm, zer, causal, alln]
    else:
        dm = [zer, causal, alln, winm, zer, causal]
    dmask = np.concatenate(dm, axis=1).astype(NP_BF16)
    cm = []
    for rr in range(9):
        r = 2 * rr + par
        cm.append(np.where(16 * k[:, None] + 31 <= 128 * r + k[None, :], 0.0, NEGB).astype(np.float32))
    cmask = np.concatenate(cm, axis=1).astype(NP_BF16)
    n = np.arange(512)
    j = np.arange(128)
    ov = ((16 * n[:, None] <= 64 * j[None, :] + 63) & (16 * n[:, None] + 31 >= 64 * j[None, :]) & (n[:, None] <= 510))
    ovl = ov.astype(np.float32).reshape(4, 128, 128).transpose(1, 0, 2).reshape(128, 512).astype(NP_BF16)
    kk = np.arange(T)
    eexp = np.where(kk[None, :] // 64 == j[:, None], 32768.0, 0.0).astype(NP_BF16)
    onehot = np.zeros((12, 12, 128), np.float32)
    for r in range(12):
        onehot[r, r, :] = 1.0
    addm = np.zeros((128, NQT, 128), np.float32)
    for qt in range(NQT):
        qi = 2 * qt + par
        t = qi * 128 + k
        cur = t // 64
        jj = j[None, :]
        forced = (jj == 0) | (jj == cur[:, None]) | (jj == cur[:, None] - 1)
        valid = jj * 64 <= t[:, None]
        addm[:, qt, :] = np.where(forced, 100.0, np.where(valid, 0.0, -100.0))
    return {
        'identb': np.eye(128, dtype=np.float32).astype(NP_BF16), 'identf': np.eye(128, dtype=np.float32),
        'eexp': eexp, 'dmask': dmask, 'cmask': cmask, 'ovl': ovl, 'onehot': onehot.reshape(12, 12 * 128),
        'addm': addm.reshape(128, NQT * 128),
    }


_NSA_CONSTS = {}


def nsa_in_maps(A, cmp_pe, cmp_w1, cmp_b1, cmp_w2, cmp_b2):
    w1 = np.ascontiguousarray(cmp_w1.reshape(2, 32, 128, 256).transpose(0, 2, 1, 3).reshape(2, 128, 8192))
    peT = np.ascontiguousarray(cmp_pe.transpose(2, 0, 1).reshape(128, 64))
    b1 = np.ascontiguousarray(cmp_b1.reshape(2, 2, 128).transpose(2, 0, 1).reshape(128, 4))
    w2 = np.ascontiguousarray(cmp_w2.reshape(2, 2, 128, 128).transpose(2, 0, 1, 3).reshape(128, 512))
    b2k = np.ascontiguousarray(cmp_b2[0][:, None])
    b2v = np.ascontiguousarray(cmp_b2[1][None, :])
    maps = []
    for c in range(NCORES):
        g, par = c // 2, c % 2
        if par not in _NSA_CONSTS:
            _NSA_CONSTS[par] = nsa_consts(par)
        m = dict(_NSA_CONSTS[par])

        def qlay(XT):
            x = XT[g * 512:(g + 1) * 512].reshape(4, 128, 64, 128)[:, :, par::2, :]
            return np.ascontiguousarray(x.transpose(1, 2, 0, 3).reshape(128, NQT * 512))
        m['qs'] = qlay(A['qaT'])
        m['sza'] = qlay(A['szaT'])
        m['gs'] = np.ascontiguousarray(A['gbrT'][g * 12:(g + 1) * 12].reshape(12, 64, 128)[:, par::2, :].reshape(12, NQT * 128))
        for nm in ('kselT', 'kwinT', 'kcmpT', 'vcmpT'):
            m[nm] = np.ascontiguousarray(A[nm][g * 128:(g + 1) * 128, :])
        for nm in ('vsel', 'vwin'):
            m[nm] = np.ascontiguousarray(A[nm][:, g * 128:(g + 1) * 128].reshape(64, 128, 128).transpose(1, 0, 2).reshape(128, T))
        m.update({'w1': w1, 'peT': peT, 'b1': b1, 'w2': w2, 'b2k': b2k, 'b2v': b2v})
        maps.append(m)
    return maps


def nsa_gather(results):
    uaT = np.zeros((2048, T), NP_BF16)
    v = uaT.reshape(4, 4, 128, 64, 128)
    for c in range(NCORES):
        g, par = c // 2, c % 2
        u = np.asarray(results[c]['ua']).reshape(128, NQT, 4, 128)
        v[g, :, :, par::2, :] = u.transpose(2, 0, 1, 3)
    return uaT


def build_gla(nst=16):
    C = Ctx()
    nc, S = C.nc, C.S
    qT_d = C.din('qT', (128, 2 * T), BF16)
    kT_d = C.din('kT', (128, 2 * T), BF16)
    kb_d = C.din('kb', (128, 64 * 256), BF16)
    vb_d = C.din('vb', (128, 64 * 256), BF16)
    alr_d = C.din('alrT', (16, T), F32)
    aw_d = C.din('aw', (16, 256), F32)
    ab_d = C.din('ab', (1, 256), F32)
    uin_d = C.din('uincl', (128, 128), F32)
    ust_d = C.din('ustrict', (128, 128), F32)
    msk_d = C.din('amask', (128, 128), F32)
    ob_d = C.dout('ob', (128, 2 * T), BF16)

    def ld(name, src, shape, dt):
        t = C.sb(name, shape, dt)
        l = S.lt(name, dma=True)
        S.dma('sp', t[:, :], src[:, :], l, writes=[l])
        return t, l
    aw, aw_lt = ld('aw', aw_d, (16, 256), F32)
    ab, ab_lt = ld('ab', ab_d, (1, 256), F32)
    uin, uin_lt = ld('uin', uin_d, (128, 128), F32)
    ust, ust_lt = ld('ust', ust_d, (128, 128), F32)
    msk, msk_lt = ld('msk', msk_d, (128, 128), F32)
    onesf = C.sb('onesf', (1, 128), F32)
    onesf_lt = S.lt('onesf')
    S.op('dve', lambda e: e.memset(onesf[:, :], 1.0), writes=[onesf_lt])

    bank = [(C.ps('bk%d' % i), S.lt('bk%d' % i)) for i in range(8)]
    ZL, CT, ATT, OT, KV = bank[0], bank[1:3], bank[3], bank[4:6], bank[6:8]

    def dbuf(name, shape, dt, dma=True):
        return [C.sb('%s%d' % (name, i), shape, dt) for i in range(2)], [S.lt('%s%d' % (name, i), dma=dma) for i in range(2)]
    qs_, qs_lt = dbuf('gq', (128, 2 * 512), BF16)
    ks_, ks_lt = dbuf('gk', (128, 2 * 512), BF16)
    kbs, kbs_lt = dbuf('gkb', (128, 4 * 256), BF16)
    vbs, vbs_lt = dbuf('gvb', (128, 4 * 256), BF16)
    als, als_lt = dbuf('gal', (16, 512), F32)
    obs, obs_lt = dbuf('gob', (128, 2 * 512), BF16)
    ez = C.sb('ez', (128, 512), F32)
    ez_lt = S.lt('ez')
    sp = C.sb('sp', (128, 4 * 256), F32)
    sp_lt = [S.lt('sp%d' % i) for i in range(2)]
    E1 = C.sb('E1', (128, 2 * 512), F32)
    E1_lt = [S.lt('E1_%d' % i) for i in range(2)]
    E2 = C.sb('E2', (128, 512), F32)
    E2_lt = S.lt('E2')
    qdec = C.sb('qdec', (128, 2 * 512), BF16)
    qdec_lt = [S.lt('qdec%d' % i) for i in range(2)]
    kinv = C.sb('kinv', (128, 2 * 512), BF16)
    kinv_lt = [S.lt('kinv%d' % i) for i in range(2)]
    E3 = C.sb('E3', (128, 512), F32)
    E3_lt = S.lt('E3')
    kst = C.sb('kst', (128, 4 * 256), BF16)
    kst_lt = [S.lt('kst%d' % i) for i in range(2)]
    attT = C.sb('attT', (128, 512), BF16)
    attT_lt = [S.lt('attT%d' % i) for i in range(4)]
    Sf = C.sb('Sf', (128, 2 * 256), F32)
    Sf_lt = [S.lt('Sf%d' % i) for i in range(2)]
    Sb = [C.sb('Sb%d' % i, (128, 2 * 256), BF16) for i in range(2)]
    Sb_lt = [[S.lt('Sb%d_%d' % (i, j)) for j in range(2)] for i in range(2)]
    S.op('dve', lambda e: e.memset(Sf[:, :], 0.0), writes=Sf_lt)
    S.op('dve', lambda e: e.memset(Sb[0][:, :], 0.0), writes=Sb_lt[0])

    qv = qT_d.rearrange("p (c t) -> p c t", c=2)
    kv_ = kT_d.rearrange("p (c t) -> p c t", c=2)
    ov = ob_d.rearrange("p (c t) -> p c t", c=2)
    n = 0
    for st in range(nst):
        b = st % 2
        t0 = st * 512
        S.dma('sp', qs_[b][:, :].rearrange("p (c t) -> p c t", c=2), qv[:, :, t0:t0 + 512], qs_lt[b], writes=[qs_lt[b]])
        S.dma('sp', ks_[b][:, :].rearrange("p (c t) -> p c t", c=2), kv_[:, :, t0:t0 + 512], ks_lt[b], writes=[ks_lt[b]])
        S.dma('sp', kbs[b][:, :], kb_d[:, st * 1024:(st + 1) * 1024], kbs_lt[b], writes=[kbs_lt[b]])
        S.dma('sp', vbs[b][:, :], vb_d[:, st * 1024:(st + 1) * 1024], vbs_lt[b], writes=[vbs_lt[b]])
        S.dma('sp', als[b][:, :], alr_d[:, t0:t0 + 512], als_lt[b], writes=[als_lt[b]])
        for tp in range(2):
            for tl in range(2):
                tile = tp * 2 + tl
                mm(S, ZL[1], ZL[0][:, tl * 256:(tl + 1) * 256], als[b][0:16, tile * 128:(tile + 1) * 128], aw[0:16, :],
                   True, False, [als_lt[b], aw_lt])
                mm(S, ZL[1], ZL[0][:, tl * 256:(tl + 1) * 256], onesf[0:1, :], ab[0:1, :], False, True,
                   [onesf_lt, ab_lt])
            act(S, ez[:, :], ZL[0][:, :], AF.Exp, [ZL[1]], [ez_lt], scale=-1.0)
            act(S, sp[:, tp * 512:(tp + 1) * 512], ez[:, :], AF.Ln, [ez_lt], [sp_lt[tp]], bias=1.0)
        for dc in range(2):
            for tile in range(4):
                mm(S, CT[dc][1], CT[dc][0][:, tile * 128:(tile + 1) * 128],
                   sp[:, tile * 256 + dc * 128: tile * 256 + (dc + 1) * 128], uin[:, :], True, True,
                   [sp_lt[tile // 2], uin_lt])
            act(S, E1[:, dc * 512:(dc + 1) * 512], CT[dc][0][:, :], AF.Exp, [CT[dc][1]], [E1_lt[dc]])
            act(S, E2[:, :], CT[dc][0][:, :], AF.Exp, [CT[dc][1]], [E2_lt], scale=-1.0)
            tt(S, 'dve', qdec[:, dc * 512:(dc + 1) * 512], qs_[b][:, dc * 512:(dc + 1) * 512], E1[:, dc * 512:(dc + 1) * 512],
               ALU.mult, [qs_lt[b], E1_lt[dc]], [qdec_lt[dc]])
            tt(S, 'dve', kinv[:, dc * 512:(dc + 1) * 512], ks_[b][:, dc * 512:(dc + 1) * 512], E2[:, :],
               ALU.mult, [ks_lt[b], E2_lt], [kinv_lt[dc]])
        for half in range(2):
            for tl in range(2):
                tile = half * 2 + tl
                mm(S, ZL[1], ZL[0][:, tl * 256:(tl + 1) * 256], ust[:, :], sp[:, tile * 256:(tile + 1) * 256], True, True,
                   [ust_lt, sp_lt[half]])
            act(S, E3[:, :], ZL[0][:, :], AF.Exp, [ZL[1]], [E3_lt])
            tt(S, 'dve', kst[:, half * 512:(half + 1) * 512], kbs[b][:, half * 512:(half + 1) * 512], E3[:, :], ALU.mult,
               [kbs_lt[b], E3_lt], [kst_lt[half]])
        for tile in range(4):
            c0 = tile * 128
            for dc in range(2):
                mm(S, ATT[1], ATT[0][:, c0:c0 + 128], kinv[:, dc * 512 + c0: dc * 512 + c0 + 128],
                   qdec[:, dc * 512 + c0: dc * 512 + c0 + 128], dc == 0, dc == 1, [kinv_lt[dc], qdec_lt[dc]])
            tt(S, 'dve', attT[:, c0:c0 + 128], ATT[0][:, c0:c0 + 128], msk[:, :], ALU.mult, [ATT[1], msk_lt],
               [attT_lt[tile]])
            for ch in range(2):
                cc = c0 + ch * 64
                sb_i = n % 2
                for vc in range(2):
                    mm(S, OT[vc][1], OT[vc][0][:, cc:cc + 64], vbs[b][:, tile * 256 + vc * 128: tile * 256 + (vc + 1) * 128],
                       attT[:, cc:cc + 64], True, False, [vbs_lt[b], attT_lt[tile]])
                    for dc in range(2):
                        mm(S, OT[vc][1], OT[vc][0][:, cc:cc + 64], Sb[sb_i][:, dc * 256 + vc * 128: dc * 256 + (vc + 1) * 128],
                           qdec[:, dc * 512 + cc: dc * 512 + cc + 64], False, dc == 1, [Sb_lt[sb_i][dc], qdec_lt[dc]])
                kvb = KV[n % 2]
                for dc in range(2):
                    mm(S, kvb[1], kvb[0][:, dc * 256:(dc + 1) * 256],
                       kst[ch * 64:(ch + 1) * 64, tile * 256 + dc * 128: tile * 256 + (dc + 1) * 128],
                       vbs[b][ch * 64:(ch + 1) * 64, tile * 256:(tile + 1) * 256], True, True,
                       [kst_lt[tile // 2], vbs_lt[b]])
                for dc in range(2):
                    S.op('dve', (lambda e, dc=dc, cc=cc, kvb=kvb: e.scalar_tensor_tensor(
                        Sf[:, dc * 256:(dc + 1) * 256], Sf[:, dc * 256:(dc + 1) * 256],
                        E1[:, dc * 512 + cc + 63: dc * 512 + cc + 64], kvb[0][:, dc * 256:(dc + 1) * 256], ALU.mult, ALU.add)),
                        reads=[Sf_lt[dc], E1_lt[dc], kvb[1]], writes=[Sf_lt[dc]])
                    act(S, Sb[1 - sb_i][:, dc * 256:(dc + 1) * 256], Sf[:, dc * 256:(dc + 1) * 256], AF.Copy,
                        [Sf_lt[dc]], [Sb_lt[1 - sb_i][dc]])
                n += 1
        for vc in range(2):
            act(S, obs[b][:, vc * 512:(vc + 1) * 512], OT[vc][0][:, :], AF.Copy, [OT[vc][1]], [obs_lt[b]])
        S.dma('sp', ov[:, :, t0:t0 + 512], obs[b][:, :].rearrange("p (c t) -> p c t", c=2), obs_lt[b], reads=[obs_lt[b]])
    return C.done(), C


def gla_consts():
    i = np.arange(128)
    same = (i[:, None] // 64) == (i[None, :] // 64)
    uincl = np.where(same & (i[:, None] <= i[None, :]), -1.0 / 16, 0.0).astype(np.float32)
    ustrict = np.where(same & (i[:, None] > i[None, :]), -1.0 / 16, 0.0).astype(np.float32)
    amask = np.where(same & (i[:, None] <= i[None, :]), 1.0, 0.0).astype(np.float32)
    return {'uincl': uincl, 'ustrict': ustrict, 'amask': amask}


def gla_in_maps(A, alpha_w, alpha_b):
    cst = gla_consts()
    maps = []
    for c in range(NCORES):
        hb, vh = c // 2, c % 2
        m = dict(cst)
        m['qT'] = np.ascontiguousarray(A['qbT'][hb * 256:(hb + 1) * 256].reshape(2, 128, T).transpose(1, 0, 2).reshape(128, 2 * T))
        m['kT'] = np.ascontiguousarray(A['kbT'][hb * 256:(hb + 1) * 256].reshape(2, 128, T).transpose(1, 0, 2).reshape(128, 2 * T))
        m['kb'] = np.ascontiguousarray(A['kb'][:, hb * 256:(hb + 1) * 256].reshape(64, 128, 256).transpose(1, 0, 2).reshape(128, 64 * 256))
        v0 = hb * 512 + vh * 256
        m['vb'] = np.ascontiguousarray(A['vb'][:, v0:v0 + 256].reshape(64, 128, 256).transpose(1, 0, 2).reshape(128, 64 * 256))
        m['alrT'] = A['alrT']
        m['aw'] = np.ascontiguousarray(alpha_w[:, hb * 256:(hb + 1) * 256])
        m['ab'] = np.ascontiguousarray(alpha_b[None, hb * 256:(hb + 1) * 256])
        maps.append(m)
    return maps


def gla_gather(results):
    obT = np.zeros((2048, T), NP_BF16)
    for c in range(NCORES):
        hb, vh = c // 2, c % 2
        o = np.asarray(results[c]['ob']).reshape(128, 2, T)
        for vc in range(2):
            r0 = hb * 512 + vh * 256 + vc * 128
            obT[r0:r0 + 128] = o[:, vc]
    return obT


def build_l3(final=False):
    C = Ctx()
    nc, S = C.nc, C.S
    uaT_d = C.din('uaT', (D, TPC), BF16)
    obT_d = C.din('obT', (D, TPC), BF16)
    szbT_d = C.din('szbT', (D, TPC), BF16)
    sgaT_d = C.din('sgaT', (D, TPC), BF16)
    sgbT_d = C.din('sgbT', (D, TPC), BF16)
    xT_d = C.din('xT', (D, TPC), F32)
    gnw_d = C.din('gnw', (128, 4), F32)
    fnw_d = C.din('fnw', (128, 16), F32)
    pa_d = C.din('p_a', (D, D), F32)
    pb_d = C.din('p_b', (D, D), F32)
    wo_d = C.din('w_out', (D, D), F32)
    xo_d = C.dout('xo', (D, TPC), F32)
    KC = 16
    NT = 512
    big = C.sb('big', (128, 2 * KC * NT), BF16)
    ua = big[:, 0:KC * NT].rearrange("p (k t) -> p k t", k=KC)
    ub = big[:, KC * NT:2 * KC * NT].rearrange("p (k t) -> p k t", k=KC)
    xn = big.bitcast(F32).rearrange("p (k t) -> p k t", k=KC)
    ua_lt = S.lt('ua', dma=True)
    ub_lt = S.lt('ub', dma=True)
    szb = [C.sb('szb%d' % i, (128, 4, NT), BF16) for i in range(2)]
    szb_lt = [S.lt('szb%d' % i, dma=True) for i in range(2)]
    yT = C.sb('yT', (128, KC, NT), BF16)
    yT_lt = S.lt('yT')
    wst = [C.sb('wst%d' % i, (128, KC, 512), F32) for i in range(2)]
    wst_lt = [S.lt('wst%d' % i, dma=True) for i in range(2)]
    wbf = [C.sb('wbf%d' % i, (128, KC, 512), BF16) for i in range(2)]
    wbf_lt = [S.lt('wbf%d' % i) for i in range(2)]
    sg = [C.sb('sg%d' % i, (128, 4, NT), BF16) for i in range(2)]
    sg_lt = [S.lt('sg%d' % i, dma=True) for i in range(2)]
    ytmp = C.sb('ytmp', (128, 4, NT), F32)
    ytmp_lt = S.lt('ytmp')
    xs = C.sb('xs', (128, 4, NT), F32)
    xs_lt = S.lt('xs', dma=True)
    gnw = C.sb('gnw', (128, 4), F32)
    gnw_lt = S.lt('gnw', dma=True)
    fnw = C.sb('fnw', (128, 16), F32)
    fnw_lt = S.lt('fnw', dma=True)
    ones = C.sb('ones', (128, 128), F32)
    ones_lt = S.lt('ones')
    sq = [C.sb('sq%d' % i, (128, NT), F32) for i in range(2)]
    sq_lt = [S.lt('sq%d' % i) for i in range(2)]
    rstd = C.sb('rstd', (128, NT), F32)
    rstd_lt = S.lt('rstd')
    psb = [(C.ps('ps%d' % i), S.lt('ps%d' % i)) for i in range(8)]
    S.dma('sp', gnw[:, :], gnw_d[:, :], gnw_lt, writes=[gnw_lt])
    S.dma('sp', fnw[:, :], fnw_d[:, :], fnw_lt, writes=[fnw_lt])
    S.op('dve', lambda e: e.memset(ones[:, :], 1.0), writes=[ones_lt])

    def rms(src3, src_lts, nk, k0, scale_div, ps):
        pst, pslt = ps
        for k in range(nk):
            i = k % 2
            act(S, sq[i][:, :], src3[:, k0 + k, :], AF.Square, src_lts, [sq_lt[i]])
            mm(S, pslt, pst[:, 0:NT], ones[:, :], sq[i][:, :], k == 0, k == nk - 1, [ones_lt, sq_lt[i]])
        ts(S, 'dve', rstd[:, :], pst[:, 0:NT], 1.0 / scale_div, EPS, ALU.mult, ALU.add, [pslt], [rstd_lt])
        act(S, rstd[:, :], rstd[:, :], AF.Sqrt, [rstd_lt], [rstd_lt])
        S.op('dve', lambda e: e.reciprocal(rstd[:, :], rstd[:, :]), reads=[rstd_lt], writes=[rstd_lt])

    for th in range(TPC // NT):
        t0 = th * NT
        uav = uaT_d.rearrange("(k p) t -> p k t", p=128)
        obv = obT_d.rearrange("(k p) t -> p k t", p=128)
        for j in range(0, KC, 4):
            S.dma('sp', ua[:, j:j + 4, :], uav[:, j:j + 4, t0:t0 + NT], ua_lt, writes=[ua_lt])
        for j in range(0, KC, 4):
            S.dma('sp', ub[:, j:j + 4, :], obv[:, j:j + 4, t0:t0 + NT], ub_lt, writes=[ub_lt])
        szv = szbT_d.rearrange("(k p) t -> p k t", p=128)
        for hb in range(4):
            sb_ = hb % 2
            S.dma('sp', szb[sb_][:, :, :], szv[:, hb * 4:(hb + 1) * 4, t0:t0 + NT], szb_lt[sb_], writes=[szb_lt[sb_]])
            rms(ub, [ub_lt], 4, hb * 4, 512.0, psb[hb % 8])
            for vc in range(4):
                k = hb * 4 + vc
                S.op('dve', (lambda e, k=k, vc=vc: e.scalar_tensor_tensor(
                    ub[:, k, :], ub[:, k, :], gnw[:, vc:vc + 1], rstd[:, :], ALU.mult, ALU.mult)),
                    reads=[ub_lt, gnw_lt, rstd_lt], writes=[ub_lt])
                tt(S, 'pool', ub[:, k, :], ub[:, k, :], szb[sb_][:, vc, :], ALU.mult, [ub_lt, szb_lt[sb_]], [ub_lt])
        sgav = sgaT_d.rearrange("(k p) t -> p k t", p=128)
        sgbv = sgbT_d.rearrange("(k p) t -> p k t", p=128)
        slabs = []

        def evac_a(extra, ncx, tt_, TT, m, pst, pslt, dst, o_lt):
            j = extra['j']
            tt(S, 'dve', ytmp[:, ncx, :], pst[:, 0:NT], sg[0][:, ncx, :], ALU.mult, [pslt, sg_lt[0]], [ytmp_lt])

        def evac_b(extra, ncx, tt_, TT, m, pst, pslt, dst, o_lt):
            j = extra['j']
            tt(S, 'dve', sq[0][:, :], pst[:, 0:NT], sg[1][:, ncx, :], ALU.mult, [pslt, sg_lt[1]], [sq_lt[0]])
            tt(S, 'pool', yT[:, j * 4 + ncx, :], sq[0][:, :], ytmp[:, ncx, :], ALU.add, [sq_lt[0], ytmp_lt], [yT_lt])

        def evac_o(extra, ncx, tt_, TT, m, pst, pslt, dst, o_lt):
            j = extra['j']
            tt(S, 'dve', xn[:, j * 4 + ncx, :], pst[:, 0:NT], xs[:, ncx, :], ALU.add, [pslt, xs_lt], [ua_lt, ub_lt])

        class Pre:
            pass
        for j in range(4):
            slabs.append((j * 512, 512, 'F', 'copy', 1.0, None, BF16,
                          {'o0': 0, 'hT': (ua, ua_lt), 'w': pa_d, 'evac': evac_a, 'nostore': True, 'j': j, 'pre': ('a', j)}))
            slabs.append((j * 512, 512, 'F', 'copy', 1.0, None, BF16,
                          {'o0': 0, 'hT': (ub, ub_lt), 'w': pb_d, 'evac': evac_b, 'nostore': True, 'j': j, 'pre': ('b', j)}))
        for j in range(4):
            slabs.append((j * 512, 512, 'F', 'copy', 1.0, None, BF16,
                          {'o0': 0, 'hT': (yT, yT_lt), 'w': wo_d, 'evac': evac_o, 'nostore': True, 'j': j, 'pre': ('o', j)}))
        xv = xT_d.rearrange("(k p) t -> p k t", p=128)

        def pre(extra):
            kind, j = extra['pre']
            if kind == 'a':
                S.dma('sp', sg[0][:, :, :], sgav[:, j * 4:(j + 1) * 4, t0:t0 + NT], sg_lt[0], writes=[sg_lt[0]])
            elif kind == 'b':
                S.dma('sp', sg[1][:, :, :], sgbv[:, j * 4:(j + 1) * 4, t0:t0 + NT], sg_lt[1], writes=[sg_lt[1]])
            else:
                S.dma('sp', xs[:, :, :], xv[:, j * 4:(j + 1) * 4, t0:t0 + NT], xs_lt, writes=[xs_lt])
        for sl in slabs:
            sl[7]['prefn'] = pre
        proj_slabs(C, ua, ua_lt, pa_d, slabs, KC, NT, wst, wst_lt, wbf, wbf_lt, psb, None, None, None, None)
        xov = xo_d.rearrange("(k p) t -> p k t", p=128)
        if final:
            rms(xn, [ua_lt, ub_lt], KC, 0, float(D), psb[0])
            for k in range(KC):
                S.op('dve', (lambda e, k=k: e.scalar_tensor_tensor(
                    xn[:, k, :], xn[:, k, :], fnw[:, k:k + 1], rstd[:, :], ALU.mult, ALU.mult)),
                    reads=[ua_lt, ub_lt, fnw_lt, rstd_lt], writes=[ua_lt, ub_lt])
        for j in range(0, KC, 4):
            S.dma('sp', xov[:, j:j + 4, t0:t0 + NT], xn[:, j:j + 4, :], ua_lt, reads=[ua_lt, ub_lt])
    return C.done(), C


_PROGS = {}


def get_prog(name):
    if name not in _PROGS:
        _PROGS[name] = {'l1': build_l1, 'nsa': build_nsa, 'gla': build_gla, 'l3': build_l3, 'l3f': (lambda: build_l3(True))}[name]()
    return _PROGS[name]


def run(name, in_maps):
    nc, C = get_prog(name)
    res = run_bass_kernel_spmd(nc, in_maps, core_ids=list(range(NCORES)))
    return res.results


def run_l1(xT_full, norm_w_l, w_in_l):
    nw = np.ascontiguousarray(norm_w_l.reshape(16, 128).T)
    in_maps = []
    for c in range(NCORES):
        in_maps.append({'xT': np.ascontiguousarray(xT_full[:, c * TPC:(c + 1) * TPC]), 'nw': nw, 'w_in': w_in_l})
    return run('l1', in_maps)


def run_layer(xT, l, P, final, log=None):
    r1 = run_l1(xT, P['norm_w'][l], P['w_in'][l])
    A = {}
    for (name, c0, ncols, layout, func, scale, dt) in L1_SPECS:
        if name in ('szbT', 'sgaT', 'sgbT'):
            continue
        A[name] = np.concatenate([np.asarray(r[name]) for r in r1], axis=1 if layout == 'F' else 0)
    r2 = run('nsa', nsa_in_maps(A, P['cmp_pe'][l], P['cmp_w1'][l], P['cmp_b1'][l], P['cmp_w2'][l], P['cmp_b2'][l]))
    uaT = nsa_gather(r2)
    r3 = run('gla', gla_in_maps(A, P['gla_alpha_w'][l], P['gla_alpha_b'][l]))
    obT = gla_gather(r3)
    if log is not None:
        log['uaT'] = uaT
        log['obT'] = obT
    gnw = np.ascontiguousarray(P['gla_norm_w'][l].reshape(4, 128).T)
    fnw = np.ascontiguousarray(P['final_norm_w'].reshape(16, 128).T)
    maps = []
    for c in range(NCORES):
        sl = slice(c * TPC, (c + 1) * TPC)
        maps.append({
            'uaT': np.ascontiguousarray(uaT[:, sl]), 'obT': np.ascontiguousarray(obT[:, sl]),
            'szbT': np.asarray(r1[c]['szbT']), 'sgaT': np.asarray(r1[c]['sgaT']), 'sgbT': np.asarray(r1[c]['sgbT']),
            'xT': np.ascontiguousarray(xT[:, sl]), 'gnw': gnw, 'fnw': fnw,
            'p_a': P['p_a'][l], 'p_b': P['p_b'][l], 'w_out': P['w_out'][l],
        })
    r4 = run('l3f' if final else 'l3', maps)
    return np.concatenate([np.asarray(r['xo']) for r in r4], axis=1)


def kernel(**inputs):
    P = {k: np.asarray(v) for k, v in inputs.items()}
    x = P['x'].astype(np.float32, copy=False)[0]
    xT = np.ascontiguousarray(x.T)
    for l in range(DEPTH):
        xT = run_layer(xT, l, P, l == DEPTH - 1)
    return np.ascontiguousarray(xT.T)[None].astype(np.float32, copy=False)
```
